# Optimizing a Trainium2 kernel written in Bass

```python
import math
import jax, jax.numpy as jnp
from jax import lax
import numpy as np

D_MODEL = 4096
BATCH = 2
SEQ = 8192
DEPTH = 2

MIX_WIDTH = D_MODEL
RWKV_WIDTH = MIX_WIDTH // 2
S5_WIDTH = MIX_WIDTH - RWKV_WIDTH
RWKV_HEAD_DIM = 64
RWKV_HEADS = RWKV_WIDTH // RWKV_HEAD_DIM
DECAY_LORA = max(32, int(round(1.8 * RWKV_WIDTH ** 0.5 / 32)) * 32)
AAA_LORA = max(32, int(round(1.8 * RWKV_WIDTH ** 0.5 / 32)) * 32)
GATE_LORA = max(32, int(round(0.6 * RWKV_WIDTH ** 0.8 / 32)) * 32)
LN_X_EPS = 64e-5
S5_GROUP_CH = 16
S5_GROUPS = S5_WIDTH // S5_GROUP_CH
S5_STATE = 64
D_FF = 4 * D_MODEL
PLE_DIM = 256
RMS_EPS = 1e-6

R_OFF = 0
K_OFF = R_OFF + RWKV_WIDTH
V_OFF = K_OFF + RWKV_WIDTH
WL_OFF = V_OFF + RWKV_WIDTH
AL_OFF = WL_OFF + DECAY_LORA
GL_OFF = AL_OFF + AAA_LORA
RWKV_COLS = GL_OFF + GATE_LORA
IN_COLS = RWKV_COLS + S5_WIDTH

kernel_name = "hybrid_rwkv7_s5_sandwich_block"


def rms_norm(x, g):
    xf = x.astype(jnp.float32)
    y = xf * lax.rsqrt(jnp.mean(xf * xf, axis=-1, keepdims=True) + RMS_EPS)
    return (y * g.astype(jnp.float32)).astype(x.dtype)


def token_shift(z):
    return jnp.pad(z[:, :-1], ((0, 0), (1, 0), (0, 0)))


def _rwkv7_recurrence(r, w, k, v, a, b):
    bsz, _, nh, nd = r.shape

    def step(S, inp):
        r_t, w_t, k_t, v_t, a_t, b_t = inp
        sa = jnp.einsum('bhij,bhj->bhi', S, a_t)
        S = (S * w_t[:, :, None, :] + sa[..., None] * b_t[:, :, None, :]
             + v_t[..., None] * k_t[:, :, None, :])
        y_t = jnp.einsum('bhij,bhj->bhi', S, r_t)
        return S, y_t

    seqs = tuple(jnp.moveaxis(t, 1, 0) for t in (r, w, k, v, a, b))
    S0 = jnp.zeros((bsz, nh, nd, nd), jnp.float32)
    _, y = lax.scan(step, S0, seqs)
    return jnp.moveaxis(y, 0, 1)


def rwkv7_mixer(zr, mu, w0, w2, a0, a2, g2, k_k, k_a, r_k, lnx_w, lnx_b):
    bsz, seq, _ = zr.shape
    zf = zr.astype(jnp.float32)
    zf = zf + (token_shift(zf) - zf) * mu.astype(jnp.float32)
    r = zf[..., R_OFF:K_OFF]
    k = zf[..., K_OFF:V_OFF]
    v = zf[..., V_OFF:WL_OFF]
    xw = zf[..., WL_OFF:AL_OFF]
    xa = zf[..., AL_OFF:GL_OFF]
    xg = zf[..., GL_OFF:RWKV_COLS]
    w_raw = -jax.nn.softplus(-(w0 + jnp.tanh(xw) @ w2)) - 0.5
    decay = jnp.exp(-jnp.exp(w_raw))
    a = jax.nn.sigmoid(a0 + xa @ a2)
    g = jax.nn.sigmoid(xg) @ g2

    def heads(t):
        return t.reshape(bsz, seq, RWKV_HEADS, RWKV_HEAD_DIM)

    kk = heads(k * k_k)
    kk = kk / jnp.maximum(jnp.sqrt(jnp.sum(kk * kk, axis=-1, keepdims=True)), 1e-12)
    k = k * (1.0 + (a - 1.0) * k_a)
    rh, kh, vh, ah = heads(r), heads(k), heads(v), heads(a)
    y = _rwkv7_recurrence(rh, heads(decay), kh, vh, -kk, kk * ah)
    mean = jnp.mean(y, axis=-1, keepdims=True)
    var = jnp.mean(jnp.square(y - mean), axis=-1, keepdims=True)
    y = ((y - mean) * lax.rsqrt(var + LN_X_EPS)).reshape(bsz, seq, RWKV_WIDTH) * lnx_w + lnx_b
    bonus = jnp.sum(rh * kh * r_k, axis=-1, keepdims=True) * vh
    y = (y + bonus.reshape(bsz, seq, RWKV_WIDTH)) * g
    return y.astype(zr.dtype)


def _complex_linear_combine(left, right):
    ar1, ai1, br1, bi1 = left
    ar2, ai2, br2, bi2 = right
    ar = ar2 * ar1 - ai2 * ai1
    ai = ar2 * ai1 + ai2 * ar1
    br = ar2 * br1 - ai2 * bi1 + br2
    bi = ar2 * bi1 + ai2 * br1 + bi2
    return (ar, ai, br, bi)


def s5_mixer(u, lam_re, lam_im, log_step, b_re, b_im, c_re, c_im, d_skip, w_glu, b_glu):
    bsz, seq, _ = u.shape
    uf = u.astype(jnp.float32)
    ug = uf.reshape(bsz, seq, S5_GROUPS, S5_GROUP_CH)
    lr = lam_re.astype(jnp.float32)
    li = lam_im.astype(jnp.float32)
    dt = jnp.exp(log_step.astype(jnp.float32))[:, None]
    mag = jnp.exp(lr * dt)
    abar_re = mag * jnp.cos(li * dt)
    abar_im = mag * jnp.sin(li * dt)
    den = lr * lr + li * li
    q_re = ((abar_re - 1.0) * lr + abar_im * li) / den
    q_im = (abar_im * lr - (abar_re - 1.0) * li) / den
    bu_re = jnp.einsum('btgh,gph->btgp', ug, b_re)
    bu_im = jnp.einsum('btgh,gph->btgp', ug, b_im)
    bx_re = q_re * bu_re - q_im * bu_im
    bx_im = q_re * bu_im + q_im * bu_re
    a_re = jnp.broadcast_to(abar_re, (1, seq, S5_GROUPS, S5_STATE))
    a_im = jnp.broadcast_to(abar_im, (1, seq, S5_GROUPS, S5_STATE))
    _, _, s_re, s_im = lax.associative_scan(
        _complex_linear_combine, (a_re, a_im, bx_re, bx_im), axis=1)
    y = (jnp.einsum('btgp,ghp->btgh', s_re, c_re)
         - jnp.einsum('btgp,ghp->btgh', s_im, c_im)
         + d_skip.reshape(S5_GROUPS, S5_GROUP_CH) * ug)
    y = jax.nn.gelu(y.reshape(bsz, seq, S5_WIDTH))
    out = y * jax.nn.sigmoid(y @ w_glu + b_glu)
    return out.astype(u.dtype)


def setup_inputs(seed: int = 0) -> dict:
    key = jax.random.key(seed)
    keys = jax.random.split(key, 40)
    counter = iter(range(40))
    L = DEPTH
    f32 = jnp.float32

    def nk():
        return keys[next(counter)]

    def nrm(shape, scale):
        return scale * jax.random.normal(nk(), shape, f32)

    def gain(shape):
        return 1.0 + nrm(shape, 0.02)

    x = nrm((BATCH, SEQ, D_MODEL), 1.0)
    p = nrm((DEPTH, BATCH, SEQ, PLE_DIM), 1.0)
    g_mix_pre = gain((L, D_MODEL))
    w_in = nrm((L, D_MODEL, IN_COLS), D_MODEL ** -0.5)
    mu = jax.random.uniform(nk(), (L, RWKV_COLS), f32, 0.2, 0.8)
    ratio = jnp.linspace(0.0, 1.0, RWKV_WIDTH, dtype=f32)
    w0 = -6.0 + 5.0 * ratio ** 0.85 + nrm((L, RWKV_WIDTH), 0.01)
    w2 = nrm((L, DECAY_LORA, RWKV_WIDTH), 0.1 * DECAY_LORA ** -0.5)
    a0 = nrm((L, RWKV_WIDTH), 0.1)
    a2 = nrm((L, AAA_LORA, RWKV_WIDTH), 0.1 * AAA_LORA ** -0.5)
    g2 = nrm((L, GATE_LORA, RWKV_WIDTH), GATE_LORA ** -0.5)
    k_k = 0.85 + nrm((L, RWKV_WIDTH), 0.02)
    k_a = 1.0 + nrm((L, RWKV_WIDTH), 0.02)
    r_k = nrm((L, RWKV_HEADS, RWKV_HEAD_DIM), 0.1)
    lnx_w = gain((L, RWKV_WIDTH))
    lnx_b = nrm((L, RWKV_WIDTH), 0.02)
    lam_re = -0.5 + nrm((L, S5_GROUPS, S5_STATE), 0.01)
    lam_im = jnp.pi * jnp.arange(S5_STATE, dtype=f32) + nrm((L, S5_GROUPS, S5_STATE), 0.01)
    log_step = jax.random.uniform(nk(), (L, S5_GROUPS), f32, math.log(1e-3), math.log(1e-1))
    b_re = nrm((L, S5_GROUPS, S5_STATE, S5_GROUP_CH), (2 * S5_GROUP_CH) ** -0.5)
    b_im = nrm((L, S5_GROUPS, S5_STATE, S5_GROUP_CH), (2 * S5_GROUP_CH) ** -0.5)
    c_re = nrm((L, S5_GROUPS, S5_GROUP_CH, S5_STATE), 0.5)
    c_im = nrm((L, S5_GROUPS, S5_GROUP_CH, S5_STATE), 0.5)
    d_skip = nrm((L, S5_WIDTH), 1.0)
    w_glu = nrm((L, S5_WIDTH, S5_WIDTH), S5_WIDTH ** -0.5)
    b_glu = nrm((L, S5_WIDTH), 0.02)
    w_out = nrm((L, MIX_WIDTH, D_MODEL), MIX_WIDTH ** -0.5)
    g_mix_post = gain((L, D_MODEL))
    g_ffn_pre = gain((L, D_MODEL))
    w_ff1 = nrm((L, D_MODEL, D_FF), D_MODEL ** -0.5)
    w_ff2 = nrm((L, D_FF, D_MODEL), D_FF ** -0.5)
    g_ffn_post = gain((L, D_MODEL))
    w_ple = nrm((L, PLE_DIM, D_MODEL), PLE_DIM ** -0.5)
    g_ple_gate = gain((L, D_MODEL))
    w_ple_gate = nrm((L, D_MODEL, D_MODEL), D_MODEL ** -0.5)
    g_ple_post = gain((L, D_MODEL))
    return {"x": x, "p": p, "g_mix_pre": g_mix_pre, "w_in": w_in, "mu": mu,
            "w0": w0, "w2": w2, "a0": a0, "a2": a2, "g2": g2, "k_k": k_k, "k_a": k_a,
            "r_k": r_k, "lnx_w": lnx_w, "lnx_b": lnx_b, "lam_re": lam_re, "lam_im": lam_im,
            "log_step": log_step, "b_re": b_re, "b_im": b_im, "c_re": c_re, "c_im": c_im,
            "d_skip": d_skip, "w_glu": w_glu, "b_glu": b_glu, "w_out": w_out,
            "g_mix_post": g_mix_post, "g_ffn_pre": g_ffn_pre, "w_ff1": w_ff1, "w_ff2": w_ff2,
            "g_ffn_post": g_ffn_post, "w_ple": w_ple, "g_ple_gate": g_ple_gate,
            "w_ple_gate": w_ple_gate, "g_ple_post": g_ple_post}


def reference(x, p, g_mix_pre, w_in, mu, w0, w2, a0, a2, g2, k_k, k_a, r_k, lnx_w, lnx_b,
              lam_re, lam_im, log_step, b_re, b_im, c_re, c_im, d_skip, w_glu, b_glu,
              w_out, g_mix_post, g_ffn_pre, w_ff1, w_ff2, g_ffn_post, w_ple, g_ple_gate,
              w_ple_gate, g_ple_post):
    h = x
    for i in range(DEPTH):
        hn = rms_norm(h, g_mix_pre[i])
        z = hn @ w_in[i]
        y_rwkv = rwkv7_mixer(z[..., :RWKV_COLS], mu[i], w0[i], w2[i], a0[i], a2[i], g2[i],
                             k_k[i], k_a[i], r_k[i], lnx_w[i], lnx_b[i])
        y_s5 = s5_mixer(z[..., RWKV_COLS:], lam_re[i], lam_im[i], log_step[i], b_re[i],
                        b_im[i], c_re[i], c_im[i], d_skip[i], w_glu[i], b_glu[i])
        mixed = jnp.concatenate([y_rwkv, y_s5], axis=-1) @ w_out[i]
        h = h + rms_norm(mixed, g_mix_post[i])
        hn = rms_norm(h, g_ffn_pre[i])
        f = jnp.square(jax.nn.relu(hn @ w_ff1[i])) @ w_ff2[i]
        h = h + rms_norm(f, g_ffn_post[i])
        e = p[i] @ w_ple[i]
        gate = jax.nn.sigmoid(rms_norm(h, g_ple_gate[i]) @ w_ple_gate[i])
        h = h + rms_norm(gate * e, g_ple_post[i])
    return h
```

```python
import numpy as np
import concourse.bass as bass
import concourse.mybir as mybir
from concourse.bass_utils import run_bass_kernel_spmd
from contextlib import ExitStack

F32 = mybir.dt.float32
BF16 = mybir.dt.bfloat16
AF = mybir.ActivationFunctionType
ALU = mybir.AluOpType
AX = mybir.AxisListType

ENGS = ['pe', 'act', 'dve', 'pool', 'sp']
NDS = 24

D_MODEL = 4096
BATCH = 2
SEQ = 8192
DEPTH = 2
NCORES = 8
RW = 2048
S5W = 2048
D_FF = 16384
PLE = 256
LN_X_EPS = 64e-5
RMS_EPS = 1e-6


class Tok:
    __slots__ = ('w', 'r', 'name')

    def __init__(self, name=''):
        self.w = None
        self.r = {}
        self.name = name


class Rec:
    def __init__(self):
        self.q = {e: [] for e in ENGS}
        self.cnt = {e: 0 for e in ENGS}
        self.waited = {e: {} for e in ENGS}
        self.ndma_k = {}

    def _deps(self, eng, reads, writes):
        need = {}
        for t in reads:
            if t.w is not None:
                k, v = t.w
                if need.get(k, 0) < v:
                    need[k] = v
        for t in writes:
            if t.w is not None and not (eng == 'pe' and t.w[0] == 'pe'):
                k, v = t.w
                if need.get(k, 0) < v:
                    need[k] = v
            for k, v in t.r.items():
                if need.get(k, 0) < v:
                    need[k] = v
        waits = []
        wd = self.waited[eng]
        for k, v in need.items():
            if wd.get(k, 0) >= v:
                continue
            wd[k] = v
            waits.append((k, v))
        return waits

    def _mark(self, ev, reads, writes):
        k, v = ev
        for t in reads:
            if t.r.get(k, 0) < v:
                t.r[k] = v
        for t in writes:
            t.w = ev
            t.r = {}

    limit = None
    total = 0

    def op(self, eng, fn, reads=(), writes=(), force=False):
        self.total += 1
        if self.limit is not None and self.total > self.limit and not force:
            return
        waits = self._deps(eng, reads, writes)
        self.cnt[eng] += 1
        ev = (eng, self.cnt[eng])
        self.q[eng].append((waits, fn, ev))
        self._mark(ev, reads, writes)

    def dma(self, eng, fn, reads=(), writes=(), force=False):
        self.total += 1
        if self.limit is not None and self.total > self.limit and not force:
            return
        kind = 'swd' if eng == 'pool' else 'dma'
        i = self.ndma_k.get(kind, 0)
        self.ndma_k[kind] = i + 1
        k = (kind, i % NDS)
        val = 16 * (i // NDS + 1)
        waits = self._deps(eng, reads, writes)
        if i >= NDS and self.waited[eng].get(k, 0) < val - 16:
            self.waited[eng][k] = val - 16
            waits.append((k, val - 16))
        ev = (k, val)
        self.q[eng].append((waits, fn, ev))
        self._mark(ev, reads, writes)

    def wait_all(self, eng, toks):
        waits = self._deps(eng, toks, ())
        self.q[eng].append((waits, None, None))

    def emit(self, nc, es):
        sems = {}
        for e in ENGS:
            sems[e] = es.enter_context(nc.semaphore('s_' + e))
        for i in range(NDS):
            sems[('dma', i)] = es.enter_context(nc.semaphore('d_%d' % i))
            sems[('swd', i)] = es.enter_context(nc.semaphore('w_%d' % i))
        block = es.enter_context(nc.Block())

        def run(engine, name):
            for waits, fn, ev in self.q[name]:
                for k, v in waits:
                    engine.wait_ge(sems[k], v)
                if fn is None:
                    continue
                ins = fn(engine)
                if ev[0] in ENGS:
                    ins.then_inc(sems[ev[0]], 1)
                else:
                    ins.then_inc(sems[ev[0]], 16)

        @block.tensor
        def _(e):
            run(e, 'pe')

        @block.scalar
        def _(e):
            run(e, 'act')

        @block.vector
        def _(e):
            run(e, 'dve')

        @block.gpsimd
        def _(e):
            run(e, 'pool')

        @block.sync
        def _(e):
            run(e, 'sp')


class Ctx:
    def __init__(self, nc, es):
        self.nc = nc
        self.es = es
        self.R = Rec()
        self.n = 0

    def sb(self, shape, dt=F32, name=None):
        self.n += 1
        return self.es.enter_context(self.nc.sbuf_tensor(name or ("sb%d" % self.n), list(shape), dt))

    def ps(self, shape, dt=F32, name=None):
        self.n += 1
        return self.es.enter_context(self.nc.psum_tensor(name or ("ps%d" % self.n), list(shape), dt))

    def din(self, name, shape, dt=F32):
        return self.nc.dram_tensor(name, list(shape), dt, kind="ExternalInput").ap()

    def dout(self, name, shape, dt=F32):
        return self.nc.dram_tensor(name, list(shape), dt, kind="ExternalOutput").ap()


def build_rwkv(T, TT=256):
    NT = T // TT
    NCH = TT // 64
    nc = bass.Bass("TRN2", target_bir_lowering=False)
    with ExitStack() as es:
        C = Ctx(nc, es)
        R = C.R
        zall = C.din("zall", [16, 128, T])
        mu_d = C.din("mu", [128, 16])
        pv_d = C.din("pv", [128, 28])
        w2_d = C.din("w2p", [128, 512])
        a2_d = C.din("a2p", [128, 512])
        g2_d = C.din("g2p", [128, 1024])
        yr = C.dout("yr", [4, 128, T])

        mu = C.sb([128, 16]); pv = C.sb([128, 28]); w2 = C.sb([128, 512]); a2 = C.sb([128, 512]); g2 = C.sb([128, 2, 512])
        omka = C.sb([128, 4])
        ident = C.sb([128, 128]); bones = C.sb([128, 128])
        m1 = C.sb([64, 128]); mask16 = C.sb([64, 16, 128]); mlow1 = C.sb([64, 64]); mlow8 = C.sb([64, 8, 64])
        segm = C.sb([128, TT])
        tP = Tok('params')
        tC = Tok('consts')
        R.dma('sp', lambda e: e.dma_start(out=mu[:], in_=mu_d), writes=[tP])
        R.dma('sp', lambda e: e.dma_start(out=pv[:], in_=pv_d), writes=[tP])
        R.dma('sp', lambda e: e.dma_start(out=w2[:], in_=w2_d), writes=[tP])
        R.dma('sp', lambda e: e.dma_start(out=a2[:], in_=a2_d), writes=[tP])
        R.dma('sp', lambda e: e.dma_start(out=g2[:], in_=g2_d.rearrange("p (k n) -> p k n", k=2)), writes=[tP])
        R.op('pool', lambda e: e.memset(ident[:], 1.0), writes=[tC])
        R.op('pool', lambda e: e.affine_select(out=ident[:], in_=ident[:], pattern=[[-1, 128]], compare_op=ALU.is_equal,
                                               fill=0.0, base=0, channel_multiplier=1), reads=[tC], writes=[tC])
        R.op('pool', lambda e: e.memset(bones[:], 0.0), writes=[tC])
        R.op('pool', lambda e: e.memset(bones[0:64, 0:64], 1.0), writes=[tC])
        R.op('pool', lambda e: e.memset(bones[64:128, 64:128], 1.0), writes=[tC])
        R.op('pool', lambda e: e.memset(m1[:], 1.0), writes=[tC])
        R.op('pool', lambda e: e.affine_select(out=m1[:, 0:64], in_=m1[:, 0:64], pattern=[[1, 64]], compare_op=ALU.is_gt,
                                               fill=0.0, base=0, channel_multiplier=-1), reads=[tC], writes=[tC])
        R.op('pool', lambda e: e.affine_select(out=m1[:, 64:128], in_=m1[:, 64:128], pattern=[[1, 64]], compare_op=ALU.is_ge,
                                               fill=0.0, base=0, channel_multiplier=-1), reads=[tC], writes=[tC])
        R.op('dve', lambda e: e.tensor_copy(out=mask16[:], in_=m1[:].unsqueeze(1).broadcast_to([64, 16, 128])), reads=[tC], writes=[tC])
        R.op('pool', lambda e: e.memset(mlow1[:], 1.0), writes=[tC])
        R.op('pool', lambda e: e.affine_select(out=mlow1[:], in_=mlow1[:], pattern=[[-1, 64]], compare_op=ALU.is_gt,
                                               fill=0.0, base=0, channel_multiplier=1), reads=[tC], writes=[tC])
        R.op('dve', lambda e: e.tensor_copy(out=mlow8[:], in_=mlow1[:].unsqueeze(1).broadcast_to([64, 8, 64])), reads=[tC], writes=[tC])
        R.op('pool', lambda e: e.memset(segm[:], 1.0), writes=[tC])
        R.op('pool', lambda e: e.memset(segm[:].rearrange("p (c s) -> p c s", s=64)[:, :, 0:1], 0.0), reads=[tC], writes=[tC])
        R.op('dve', lambda e: e.tensor_scalar(out=omka[:], in0=pv[:, 12:16], scalar1=-1.0, scalar2=1.0, op0=ALU.mult, op1=ALU.add),
             reads=[tP], writes=[tP])

        def pcol(i, ct):
            return pv[:, i * 4 + ct:i * 4 + ct + 1]

        zbuf = C.sb([128, 16, TT + 1]); zs = C.sb([128, 16, TT])
        tw = C.sb([128, TT]); sg = C.sb([128, 2, TT])
        gg = C.sb([128, 4, TT]); KT = C.sb([128, 4, TT]); BT = C.sb([128, 4, TT]); AR = C.sb([128, 4, NCH, 2, 64])
        EP = C.sb([128, 4, TT]); bonus = C.sb([128, 4, TT]); ynfm = C.sb([128, 4, TT]); yout = C.sb([128, 4, TT])
        s_sig = C.sb([128, TT]); s_a = C.sb([128, TT]); s_kk0 = C.sb([128, TT]); s_sq = C.sb([128, TT]); s_rn = C.sb([128, TT])
        s_kk = C.sb([128, TT]); s_fac = C.sb([128, TT]); s_k2 = C.sb([128, TT]); s_bv = C.sb([128, TT]); s_lw = C.sb([128, TT])
        s_cl = C.sb([128, TT]); s_en = C.sb([128, TT]); s_ex = C.sb([128, TT]); s_t = C.sb([128, TT]); s_pr = C.sb([128, TT])
        ST = C.sb([64, 8, 64]); tmpS = C.sb([64, 8, 64])
        KT1 = C.sb([64, 4, TT]); BT1 = C.sb([64, 4, TT]); AR1 = C.sb([64, 4, NCH, 2, 64]); Gc = C.sb([128, 4, NCH]); G1 = C.sb([64, 4, NCH])
        V_sb = C.sb([64, 4, 128]); BK_tm = C.sb([64, 8, 128]); L_sb = C.sb([64, 8, 2, 128]); Q0 = C.sb([64, 8, 64])
        PQ = [C.sb([64, 8, 128]) for _ in range(2)]
        R_sb = C.sb([64, 8, 64]); W_sb = C.sb([64, 8, 64]); U_sb = C.sb([64, 8, 64]); Y_sb = C.sb([64, 8, 64])
        cent = C.sb([64, 8, 64]); sqy = C.sb([64, 8, 64]); st1 = C.sb([64, 8]); st2 = C.sb([64, 8]); yn = C.sb([64, 8, 64])
        big0 = C.ps([128, 1024]); big1 = C.ps([128, 1024])
        sm = [C.ps([128, 512]) for _ in range(4)]
        t_big0, t_big1 = Tok('big0'), Tok('big1')
        t_sm = [Tok('sm%d' % i) for i in range(4)]
        names = ['zbuf', 'zs', 'tw', 'sg', 'gg', 'KT', 'BT', 'AR', 'EP', 'bonus', 'ynfm', 'yout', 'sig', 'a', 'kk0', 'sq', 'rn',
                 'kk', 'fac', 'k2', 'bv', 'lw', 'cl', 'en', 'ex', 't', 'pr', 'ST', 'tmpS', 'V', 'BK', 'L', 'Q0', 'PQ0', 'PQ1',
                 'Rm', 'W', 'U', 'Y', 'cent', 'sqy', 'st1', 'st2', 'yn', 'yrd', 'KT1', 'BT1', 'AR1', 'Gc', 'G1']
        k = {n: Tok(n) for n in names}

        R.op('dve', lambda e: e.memset(ST[:], 0.0), writes=[k['ST']])
        R.op('dve', lambda e: e.memset(zbuf[:, :, 0:1], 0.0), writes=[k['zbuf']])

        psa = [0]

        def next_sm():
            i = psa[0] % 2
            psa[0] += 1
            return sm[i], t_sm[i]

        for it in range(NT):
            t0 = it * TT
            if it > 0:
                R.op('dve', lambda e: e.tensor_copy(out=zbuf[:, :, 0:1], in_=zbuf[:, :, TT:TT + 1]), reads=[k['zbuf']], writes=[k['zbuf']])
            for j in range(16):
                R.dma('sp', lambda e, t0=t0, j=j: e.dma_start(out=zbuf[:, j, 1:TT + 1], in_=zall[j, :, t0:t0 + TT]), writes=[k['zbuf']])
            R.op('dve', lambda e: e.tensor_tensor(out=zs[:], in0=zbuf[:, :, 0:TT], in1=zbuf[:, :, 1:TT + 1], op=ALU.subtract),
                 reads=[k['zbuf']], writes=[k['zs']])
            for j in range(16):
                R.op('dve', lambda e, j=j: e.scalar_tensor_tensor(out=zs[:, j, :], in0=zs[:, j, :], scalar=mu[:, j:j + 1],
                                                                  in1=zbuf[:, j, 1:TT + 1], op0=ALU.mult, op1=ALU.add),
                     reads=[k['zs'], k['zbuf'], tP], writes=[k['zs']])
            R.op('act', lambda e: e.activation(out=tw[:], in_=zs[:, 12, :], func=AF.Tanh), reads=[k['zs']], writes=[k['tw']])
            R.op('act', lambda e: e.activation(out=sg[:], in_=zs[:, 14:16, :], func=AF.Sigmoid), reads=[k['zs']], writes=[k['sg']])
            for ct in range(4):
                p, tp = next_sm()
                R.op('pe', lambda e, p=p, ct=ct: e.matmul(p[:, 0:TT], w2[:, ct * 128:(ct + 1) * 128], tw[:], start=True, stop=True),
                     reads=[tP, k['tw']], writes=[tp])
                R.op('act', lambda e, p=p, ct=ct: e.activation(out=s_sig[:], in_=p[:, 0:TT], func=AF.Sigmoid, bias=pcol(0, ct)),
                     reads=[tp, tP], writes=[k['sig']])
                p, tp = next_sm()
                R.op('pe', lambda e, p=p, ct=ct: e.matmul(p[:, 0:TT], a2[:, ct * 128:(ct + 1) * 128], zs[:, 13, :], start=True, stop=True),
                     reads=[tP, k['zs']], writes=[tp])
                R.op('act', lambda e, p=p, ct=ct: e.activation(out=s_a[:], in_=p[:, 0:TT], func=AF.Sigmoid, bias=pcol(1, ct)),
                     reads=[tp, tP], writes=[k['a']])
                p, tp = next_sm()
                R.op('pe', lambda e, p=p, ct=ct: e.matmul(p[:, 0:TT], g2[:, 0, ct * 128:(ct + 1) * 128], sg[:, 0, :], start=True, stop=False),
                     reads=[tP, k['sg']], writes=[tp])
                R.op('pe', lambda e, p=p, ct=ct: e.matmul(p[:, 0:TT], g2[:, 1, ct * 128:(ct + 1) * 128], sg[:, 1, :], start=False, stop=True),
                     reads=[tP, k['sg']], writes=[tp])
                R.op('act', lambda e, p=p, ct=ct: e.activation(out=gg[:, ct, :], in_=p[:, 0:TT], func=AF.Copy), reads=[tp], writes=[k['gg']])
                R.op('dve', lambda e, ct=ct: e.tensor_scalar(out=s_kk0[:], in0=zs[:, 4 + ct, :], scalar1=pcol(2, ct), scalar2=None, op0=ALU.mult),
                     reads=[k['zs'], tP], writes=[k['kk0']])
                R.op('dve', lambda e: e.tensor_tensor(out=s_sq[:], in0=s_kk0[:], in1=s_kk0[:], op=ALU.mult), reads=[k['kk0']], writes=[k['sq']])
                p, tp = next_sm()
                R.op('pe', lambda e, p=p: e.matmul(p[:, 0:TT], bones[:], s_sq[:], start=True, stop=True), reads=[tC, k['sq']], writes=[tp])
                R.op('dve', lambda e, p=p: e.tensor_scalar(out=s_rn[:], in0=p[:, 0:TT], scalar1=1e-24, scalar2=None, op0=ALU.max),
                     reads=[tp], writes=[k['rn']])
                R.op('act', lambda e: e.activation(out=s_rn[:], in_=s_rn[:], func=AF.Sqrt), reads=[k['rn']], writes=[k['rn']])
                R.op('dve', lambda e: e.reciprocal(out=s_rn[:], in_=s_rn[:]), reads=[k['rn']], writes=[k['rn']])
                R.op('dve', lambda e: e.tensor_tensor(out=s_kk[:], in0=s_kk0[:], in1=s_rn[:], op=ALU.mult), reads=[k['kk0'], k['rn']], writes=[k['kk']])
                R.op('dve', lambda e, ct=ct: e.tensor_scalar(out=s_fac[:], in0=s_a[:], scalar1=pcol(3, ct), scalar2=omka[:, ct:ct + 1],
                                                             op0=ALU.mult, op1=ALU.add), reads=[k['a'], tP], writes=[k['fac']])
                R.op('dve', lambda e, ct=ct: e.tensor_tensor(out=s_k2[:], in0=zs[:, 4 + ct, :], in1=s_fac[:], op=ALU.mult),
                     reads=[k['zs'], k['fac']], writes=[k['k2']])
                R.op('dve', lambda e: e.tensor_tensor(out=s_bv[:], in0=s_kk[:], in1=s_a[:], op=ALU.mult), reads=[k['kk'], k['a']], writes=[k['bv']])
                R.op('dve', lambda e: e.tensor_scalar(out=s_lw[:], in0=s_sig[:], scalar1=-0.6065306597126334, scalar2=None, op0=ALU.mult),
                     reads=[k['sig']], writes=[k['lw']])
                R.op('dve', lambda e: e.tensor_tensor_scan(out=s_cl[:], data0=segm[:], data1=s_lw[:], initial=0.0, op0=ALU.mult, op1=ALU.add),
                     reads=[k['lw'], tC], writes=[k['cl']])
                R.op('act', lambda e, ct=ct: e.activation(out=EP[:, ct, :], in_=s_cl[:], func=AF.Exp), reads=[k['cl']], writes=[k['EP']])
                R.op('act', lambda e: e.activation(out=s_en[:], in_=s_cl[:], func=AF.Exp, scale=-1.0), reads=[k['cl']], writes=[k['en']])
                R.op('dve', lambda e: e.tensor_tensor(out=s_t[:], in0=s_cl[:], in1=s_lw[:], op=ALU.subtract), reads=[k['cl'], k['lw']], writes=[k['t']])
                R.op('act', lambda e: e.activation(out=s_ex[:], in_=s_t[:], func=AF.Exp), reads=[k['t']], writes=[k['ex']])
                R.op('dve', lambda e, ct=ct: e.tensor_tensor(out=AR[:, ct, :, 1, :], in0=zs[:, ct, :].rearrange("p (c s) -> p c s", s=64),
                                                             in1=EP[:, ct, :].rearrange("p (c s) -> p c s", s=64), op=ALU.mult),
                     reads=[k['zs'], k['EP']], writes=[k['AR']])
                R.op('dve', lambda e, ct=ct: e.scalar_tensor_tensor(out=AR[:, ct, :, 0, :], in0=s_kk[:].rearrange("p (c s) -> p c s", s=64),
                                                                    scalar=-1.0, in1=s_ex[:].rearrange("p (c s) -> p c s", s=64),
                                                                    op0=ALU.mult, op1=ALU.mult),
                     reads=[k['kk'], k['ex']], writes=[k['AR']])
                R.op('dve', lambda e, ct=ct: e.tensor_tensor(out=KT[:, ct, :], in0=s_k2[:], in1=s_en[:], op=ALU.mult),
                     reads=[k['k2'], k['en']], writes=[k['KT']])
                R.op('dve', lambda e, ct=ct: e.tensor_tensor(out=BT[:, ct, :], in0=s_bv[:], in1=s_en[:], op=ALU.mult),
                     reads=[k['bv'], k['en']], writes=[k['BT']])
                R.op('dve', lambda e, ct=ct: e.scalar_tensor_tensor(out=s_pr[:], in0=zs[:, ct, :], scalar=pcol(6, ct), in1=s_k2[:],
                                                                    op0=ALU.mult, op1=ALU.mult), reads=[k['zs'], k['k2'], tP], writes=[k['pr']])
                p, tp = next_sm()
                R.op('pe', lambda e, p=p: e.matmul(p[:, 0:TT], bones[:], s_pr[:], start=True, stop=True), reads=[tC, k['pr']], writes=[tp])
                R.op('dve', lambda e, p=p, ct=ct: e.tensor_tensor(out=bonus[:, ct, :], in0=p[:, 0:TT], in1=zs[:, 8 + ct, :], op=ALU.mult),
                     reads=[tp, k['zs']], writes=[k['bonus']])

            R.op('dve', lambda e: e.tensor_copy(out=Gc[:], in_=EP[:].rearrange("p a (c s) -> p a c s", s=64)[:, :, :, 63]),
                 reads=[k['EP']], writes=[k['Gc']])
            R.dma('sp', lambda e: e.dma_start(out=KT1[:], in_=KT[64:128, :, :]), reads=[k['KT']], writes=[k['KT1']])
            R.dma('sp', lambda e: e.dma_start(out=BT1[:], in_=BT[64:128, :, :]), reads=[k['BT']], writes=[k['BT1']])
            R.dma('sp', lambda e: e.dma_start(out=AR1[:], in_=AR[64:128]), reads=[k['AR']], writes=[k['AR1']])
            R.dma('sp', lambda e: e.dma_start(out=G1[:], in_=Gc[64:128, :, :]), reads=[k['Gc']], writes=[k['G1']])

            def kt_(h, cs):
                return (KT[0:64, h // 2, cs], k['KT']) if h % 2 == 0 else (KT1[:, h // 2, cs], k['KT1'])

            def bt_(h, cs):
                return (BT[0:64, h // 2, cs], k['BT']) if h % 2 == 0 else (BT1[:, h // 2, cs], k['BT1'])

            def ar_(h, c, m=None):
                src, tk = (AR[0:64], k['AR']) if h % 2 == 0 else (AR1, k['AR1'])
                if m is None:
                    return src[:, h // 2, c, :, :].rearrange("p a b -> p (a b)"), tk
                return src[:, h // 2, c, m, :], tk

            for c in range(NCH):
                cs = slice(c * 64, (c + 1) * 64)
                for ct in range(4):
                    R.op('pe', lambda e, ct=ct, cs=cs: e.transpose(out=sm[2][0:64, ct * 128:(ct + 1) * 128], in_=zs[:, 8 + ct, cs], identity=ident[:]),
                         reads=[k['zs'], tC], writes=[t_sm[2]])
                R.op('act', lambda e: e.activation(out=V_sb[:], in_=sm[2][0:64, :].rearrange("p (a b) -> p a b", b=128), func=AF.Copy),
                     reads=[t_sm[2]], writes=[k['V']])
                for ct in range(4):
                    R.op('pe', lambda e, ct=ct, cs=cs: e.transpose(out=big0[0:64, ct * 128:(ct + 1) * 128], in_=BT[:, ct, cs], identity=ident[:]),
                         reads=[k['BT'], tC], writes=[t_big0])
                    R.op('pe', lambda e, ct=ct, cs=cs: e.transpose(out=big0[0:64, (4 + ct) * 128:(5 + ct) * 128], in_=KT[:, ct, cs], identity=ident[:]),
                         reads=[k['KT'], tC], writes=[t_big0])
                R.op('act', lambda e: e.activation(out=BK_tm[:], in_=big0[0:64, :].rearrange("p (a b) -> p a b", b=128), func=AF.Copy),
                     reads=[t_big0], writes=[k['BK']])
                for h in range(8):
                    pp, tpp = (big1, t_big1) if h < 4 else (big0, t_big0)
                    o = (h % 4) * 256
                    (kt_a, kt_t), (bt_a, bt_t), (ar_a, ar_t), (a0_a, _) = kt_(h, cs), bt_(h, cs), ar_(h, c), ar_(h, c, 0)
                    R.op('pe', lambda e, pp=pp, o=o, x=kt_a, y=ar_a: e.matmul(pp[0:64, o:o + 128], x, y, start=True, stop=True),
                         reads=[kt_t, ar_t], writes=[tpp])
                    R.op('pe', lambda e, pp=pp, o=o, x=bt_a, y=ar_a: e.matmul(pp[0:64, o + 128:o + 256], x, y, start=True, stop=True),
                         reads=[bt_t, ar_t], writes=[tpp])
                    R.op('pe', lambda e, h=h, x=a0_a, y=bt_a: e.matmul(sm[3][0:64, h * 64:(h + 1) * 64], x, y, start=True, stop=True),
                         reads=[bt_t, ar_t], writes=[t_sm[3]])
                R.op('dve', lambda e: e.tensor_tensor(out=L_sb[:, 0:4, :, :].rearrange("p h m x -> p (h m) x"),
                                                      in0=big1[0:64, :].rearrange("p (a b) -> p a b", b=128), in1=mask16[:, 0:8, :], op=ALU.mult),
                     reads=[t_big1, tC], writes=[k['L']])
                R.op('dve', lambda e: e.tensor_tensor(out=L_sb[:, 4:8, :, :].rearrange("p h m x -> p (h m) x"),
                                                      in0=big0[0:64, :].rearrange("p (a b) -> p a b", b=128), in1=mask16[:, 8:16, :], op=ALU.mult),
                     reads=[t_big0, tC], writes=[k['L']])
                R.op('dve', lambda e: e.tensor_tensor(out=Q0[:], in0=sm[3][0:64, :].rearrange("p (a b) -> p a b", b=64), in1=mlow8[:], op=ALU.mult),
                     reads=[t_sm[3], tC], writes=[k['Q0']])
                R.op('dve', lambda e: e.tensor_tensor(out=R_sb[:], in0=L_sb[:, :, 1, 0:64],
                                                      in1=ident[0:64, 0:64].unsqueeze(1).broadcast_to([64, 8, 64]), op=ALU.add),
                     reads=[k['L'], tC], writes=[k['Rm']])
                for lv in range(1, 6):
                    cur = PQ[lv % 2]
                    tcur = k['PQ%d' % (lv % 2)]
                    prv = PQ[(lv - 1) % 2]
                    tprv = k['PQ%d' % ((lv - 1) % 2)]
                    pq_ps, t_pq = (big1, t_big1) if lv % 2 == 1 else (big0, t_big0)
                    for h in range(8):
                        if lv == 1:
                            Pp = L_sb[:, h, 1, 0:64]
                            Qp = Q0[:, h, :]
                            rd = [k['L'], k['Q0']]
                        else:
                            Pp = prv[:, h, 0:64]
                            Qp = prv[:, h, 64:128]
                            rd = [tprv]
                        if lv < 5:
                            R.op('pe', lambda e, pq_ps=pq_ps, h=h, Pp=Pp, Qp=Qp: e.matmul(pq_ps[0:64, h * 128:h * 128 + 64], Qp, Pp, start=True, stop=True),
                                 reads=rd, writes=[t_pq])
                        R.op('pe', lambda e, pq_ps=pq_ps, h=h, Pp=Pp, Qp=Qp: e.matmul(pq_ps[0:64, h * 128 + 64:h * 128 + 128], Pp, Qp, start=True, stop=True),
                             reads=rd, writes=[t_pq])
                    if lv < 5:
                        R.op('act', lambda e, cur=cur, pq_ps=pq_ps: e.activation(out=cur[:], in_=pq_ps[0:64, :].rearrange("p (a b) -> p a b", b=128),
                                                                                 func=AF.Copy), reads=[t_pq], writes=[tcur])
                    else:
                        R.op('act', lambda e, cur=cur, pq_ps=pq_ps: e.activation(out=cur[:, :, 64:128],
                                                                                 in_=pq_ps[0:64, :].rearrange("p (a b) -> p a b", b=128)[:, :, 64:128],
                                                                                 func=AF.Copy), reads=[t_pq], writes=[tcur])
                    for h in range(8):
                        R.op('pe', lambda e, cur=cur, h=h: e.matmul(sm[3][0:64, h * 64:(h + 1) * 64], cur[:, h, 64:128], R_sb[:, h, :], start=True, stop=True),
                             reads=[tcur, k['Rm']], writes=[t_sm[3]])
                    R.op('dve', lambda e: e.tensor_tensor(out=R_sb[:], in0=R_sb[:], in1=sm[3][0:64, :].rearrange("p (a b) -> p a b", b=64), op=ALU.add),
                         reads=[t_sm[3], k['Rm']], writes=[k['Rm']])
                for h in range(8):
                    ct, hh = h // 2, h % 2
                    a0_a, ar_t = ar_(h, c, 0)
                    R.op('pe', lambda e, h=h, x=a0_a: e.matmul(sm[0][0:64, h * 64:(h + 1) * 64], x, ST[:, h, :], start=True, stop=False),
                         reads=[ar_t, k['ST']], writes=[t_sm[0]])
                    R.op('pe', lambda e, h=h, ct=ct, hh=hh: e.matmul(sm[0][0:64, h * 64:(h + 1) * 64], L_sb[:, h, 0, 0:64], V_sb[:, ct, hh * 64:(hh + 1) * 64],
                                                                   start=False, stop=True),
                         reads=[k['L'], k['V']], writes=[t_sm[0]])
                R.op('act', lambda e: e.activation(out=W_sb[:], in_=sm[0][0:64, :].rearrange("p (a b) -> p a b", b=64), func=AF.Copy),
                     reads=[t_sm[0]], writes=[k['W']])
                for h in range(8):
                    R.op('pe', lambda e, h=h: e.matmul(sm[1][0:64, h * 64:(h + 1) * 64], R_sb[:, h, :], W_sb[:, h, :], start=True, stop=True),
                         reads=[k['Rm'], k['W']], writes=[t_sm[1]])
                R.op('act', lambda e: e.activation(out=U_sb[:], in_=sm[1][0:64, :].rearrange("p (a b) -> p a b", b=64), func=AF.Copy),
                     reads=[t_sm[1]], writes=[k['U']])
                for h in range(8):
                    ct, hh = h // 2, h % 2
                    r_a, ar_t = ar_(h, c, 1)
                    R.op('pe', lambda e, h=h, x=r_a: e.matmul(sm[2][0:64, h * 64:(h + 1) * 64], x, ST[:, h, :], start=True, stop=False),
                         reads=[ar_t, k['ST']], writes=[t_sm[2]])
                    R.op('pe', lambda e, h=h: e.matmul(sm[2][0:64, h * 64:(h + 1) * 64], L_sb[:, h, 1, 64:128], U_sb[:, h, :], start=False, stop=False),
                         reads=[k['L'], k['U']], writes=[t_sm[2]])
                    R.op('pe', lambda e, h=h, ct=ct, hh=hh: e.matmul(sm[2][0:64, h * 64:(h + 1) * 64], L_sb[:, h, 0, 64:128], V_sb[:, ct, hh * 64:(hh + 1) * 64],
                                                                   start=False, stop=True),
                         reads=[k['L'], k['V']], writes=[t_sm[2]])
                R.op('act', lambda e: e.activation(out=Y_sb[:], in_=sm[2][0:64, :].rearrange("p (a b) -> p a b", b=64), func=AF.Copy),
                     reads=[t_sm[2]], writes=[k['Y']])
                for h in range(8):
                    ct, hh = h // 2, h % 2
                    R.op('pe', lambda e, h=h, ct=ct, hh=hh: e.matmul(sm[0][0:64, h * 64:(h + 1) * 64], BK_tm[:, ct, hh * 64:(hh + 1) * 64], U_sb[:, h, :],
                                                                   start=True, stop=False),
                         reads=[k['BK'], k['U']], writes=[t_sm[0]])
                    R.op('pe', lambda e, h=h, ct=ct, hh=hh: e.matmul(sm[0][0:64, h * 64:(h + 1) * 64], BK_tm[:, 4 + ct, hh * 64:(hh + 1) * 64],
                                                                   V_sb[:, ct, hh * 64:(hh + 1) * 64], start=False, stop=True),
                         reads=[k['BK'], k['V']], writes=[t_sm[0]])
                R.op('dve', lambda e: e.tensor_tensor(out=tmpS[:], in0=sm[0][0:64, :].rearrange("p (a b) -> p a b", b=64), in1=ST[:], op=ALU.add),
                     reads=[t_sm[0], k['ST']], writes=[k['tmpS']])
                for hh in range(2):
                    gsrc, gt = (Gc[0:64], k['Gc']) if hh == 0 else (G1, k['G1'])
                    R.op('dve', lambda e, hh=hh, gsrc=gsrc, c=c: e.tensor_tensor(
                        out=ST[:].rearrange("p (a b) v -> p a b v", b=2)[:, :, hh, :],
                        in0=tmpS[:].rearrange("p (a b) v -> p a b v", b=2)[:, :, hh, :],
                        in1=gsrc[:, :, c:c + 1].broadcast_to([64, 4, 64]), op=ALU.mult),
                         reads=[k['tmpS'], gt], writes=[k['ST']])
                R.op('dve', lambda e: e.tensor_reduce(out=st1[:], in_=Y_sb[:], axis=AX.X, op=ALU.add), reads=[k['Y']], writes=[k['st1']])
                R.op('dve', lambda e: e.tensor_scalar(out=st1[:], in0=st1[:], scalar1=1.0 / 64, scalar2=None, op0=ALU.mult), reads=[k['st1']], writes=[k['st1']])
                R.op('dve', lambda e: e.tensor_tensor(out=cent[:], in0=Y_sb[:], in1=st1[:].unsqueeze(2).broadcast_to([64, 8, 64]), op=ALU.subtract),
                     reads=[k['Y'], k['st1']], writes=[k['cent']])
                R.op('dve', lambda e: e.tensor_tensor(out=sqy[:], in0=cent[:], in1=cent[:], op=ALU.mult), reads=[k['cent']], writes=[k['sqy']])
                R.op('dve', lambda e: e.tensor_reduce(out=st2[:], in_=sqy[:], axis=AX.X, op=ALU.add), reads=[k['sqy']], writes=[k['st2']])
                R.op('dve', lambda e: e.tensor_scalar(out=st2[:], in0=st2[:], scalar1=1.0 / 64, scalar2=LN_X_EPS, op0=ALU.mult, op1=ALU.add),
                     reads=[k['st2']], writes=[k['st2']])
                R.op('act', lambda e: e.activation(out=st2[:], in_=st2[:], func=AF.Sqrt), reads=[k['st2']], writes=[k['st2']])
                R.op('dve', lambda e: e.reciprocal(out=st2[:], in_=st2[:]), reads=[k['st2']], writes=[k['st2']])
                R.op('dve', lambda e: e.tensor_tensor(out=yn[:], in0=cent[:], in1=st2[:].unsqueeze(2).broadcast_to([64, 8, 64]), op=ALU.mult),
                     reads=[k['cent'], k['st2']], writes=[k['yn']])
                for ct in range(4):
                    R.op('pe', lambda e, ct=ct: e.transpose(out=sm[1][:, ct * 64:(ct + 1) * 64], in_=yn[:, 2 * ct:2 * ct + 2, :].rearrange("p a b -> p (a b)"),
                                                            identity=ident[0:64, 0:64]),
                         reads=[k['yn'], tC], writes=[t_sm[1]])
                for ct in range(4):
                    R.op('dve', lambda e, ct=ct, cs=cs: e.tensor_scalar(out=ynfm[:, ct, cs], in0=sm[1][:, ct * 64:(ct + 1) * 64], scalar1=pcol(4, ct),
                                                                        scalar2=pcol(5, ct), op0=ALU.mult, op1=ALU.add),
                         reads=[t_sm[1], tP], writes=[k['ynfm']])
            R.op('dve', lambda e: e.tensor_tensor(out=yout[:], in0=ynfm[:], in1=bonus[:], op=ALU.add), reads=[k['ynfm'], k['bonus']], writes=[k['yout']])
            R.op('dve', lambda e: e.tensor_tensor(out=yout[:], in0=yout[:], in1=gg[:], op=ALU.mult), reads=[k['yout'], k['gg']], writes=[k['yout']])
            for j in range(4):
                R.dma('sp', lambda e, t0=t0, j=j: e.dma_start(out=yr[j, :, t0:t0 + TT], in_=yout[:, j, :]), reads=[k['yout']], writes=[k['yrd']])
        R.wait_all('sp', [k['yrd']])
        R.emit(nc, es)
    return nc


def _tile_rows(a, nt):
    return np.ascontiguousarray(a.reshape(nt, 128, a.shape[-1]))


def _pad_rows(a, n):
    out = np.zeros((n,) + a.shape[1:], a.dtype)
    out[:a.shape[0]] = a
    return out


def _cols(v, nt):
    return np.ascontiguousarray(v.reshape(nt, 128).T)


def rwkv_core_inputs(zr, zk, zv, zw, za, zg, lp, hg):
    sl = slice(hg * 512, (hg + 1) * 512)
    T = zr.shape[-1]
    zall = np.empty((16, 128, T), np.float32)
    zall[0:4] = zr[sl].reshape(4, 128, T)
    zall[4:8] = zk[sl].reshape(4, 128, T)
    zall[8:12] = zv[sl].reshape(4, 128, T)
    zall[12] = _pad_rows(zw, 128)
    zall[13] = _pad_rows(za, 128)
    zall[14:16] = zg.reshape(2, 128, T)
    mu = lp['mu']
    mu16 = np.zeros((128, 16), np.float32)
    mu16[:, 0:4] = _cols(mu[0:2048][sl], 4)
    mu16[:, 4:8] = _cols(mu[2048:4096][sl], 4)
    mu16[:, 8:12] = _cols(mu[4096:6144][sl], 4)
    mu16[:96, 12] = mu[6144:6240]
    mu16[:96, 13] = mu[6240:6336]
    mu16[:, 14:16] = _cols(mu[6336:6592], 2)
    pv = np.zeros((128, 28), np.float32)
    for i, nme in enumerate(['w0', 'a0', 'k_k', 'k_a', 'lnx_w', 'lnx_b']):
        pv[:, i * 4:(i + 1) * 4] = _cols(lp[nme][sl], 4)
    pv[:, 24:28] = _cols(lp['r_k'].reshape(-1)[sl], 4)
    w2p = _pad_rows(np.ascontiguousarray(lp['w2'][:, sl]), 128)
    a2p = _pad_rows(np.ascontiguousarray(lp['a2'][:, sl]), 128)
    g2p = np.ascontiguousarray(lp['g2'][:, sl].reshape(2, 128, 512).transpose(1, 0, 2).reshape(128, 1024))
    return {"zall": zall, "mu": mu16, "pv": pv, "w2p": w2p, "a2p": a2p, "g2p": g2p}


def build_s5(T, TT=512):
    NT = T // TT
    CS = 128
    NQ = TT // CS
    nc = bass.Bass("TRN2", target_bir_lowering=False)
    with ExitStack() as es:
        C = Ctx(nc, es)
        R = C.R
        zu = C.din("zu", [4, 128, T])
        lam_d = C.din("lam", [128, 48])
        Bt_d = C.din("Bt", [128, 16 * 2 * 128])
        Ct_d = C.din("Ct", [128, 16 * 2 * 128])
        dsk_d = C.din("dsk", [128, 4])
        ys = C.dout("ys", [4, 128, T])

        lam = C.sb([128, 3, 16]); Bt = C.sb([128, 16, 2, 128]); Ct = C.sb([128, 16, 2, 128]); dsk = C.sb([128, 4])
        tP = Tok('P')
        R.dma('sp', lambda e: e.dma_start(out=lam[:], in_=lam_d.rearrange("p (a b) -> p a b", a=3)), writes=[tP])
        R.dma('sp', lambda e: e.dma_start(out=Bt[:], in_=Bt_d.rearrange("p (a b c) -> p a b c", a=16, b=2)), writes=[tP])
        R.dma('sp', lambda e: e.dma_start(out=Ct[:], in_=Ct_d.rearrange("p (a b c) -> p a b c", a=16, b=2)), writes=[tP])
        R.dma('sp', lambda e: e.dma_start(out=dsk[:], in_=dsk_d), writes=[tP])
        R.op('dve', lambda e: e.tensor_scalar(out=Ct[:, :, 1, :], in0=Ct[:, :, 1, :], scalar1=-1.0, scalar2=None, op0=ALU.mult), reads=[tP], writes=[tP])

        def small(n=16):
            return C.sb([128, n])
        tS = Tok('setup')
        dt_ = small(); lrdt = small(); lidt = small(); mag = small(); sn = small(); cs_ = small(); t1 = small(); t2 = small()
        are = small(); aim = small(); den = small(); am1 = small(); qre = small(); qim = small(); ire = small(); iim = small(); im2 = small()
        lr = lam[:, 0, :]; li = lam[:, 1, :]; ls = lam[:, 2, :]

        def D(fn, **kw):
            R.op('dve', fn, reads=[tS, tP], writes=[tS])

        def A(fn):
            R.op('act', fn, reads=[tS, tP], writes=[tS])
        A(lambda e: e.activation(out=dt_[:], in_=ls, func=AF.Exp))
        D(lambda e: e.tensor_tensor(out=lrdt[:], in0=lr, in1=dt_[:], op=ALU.mult))
        D(lambda e: e.tensor_tensor(out=lidt[:], in0=li, in1=dt_[:], op=ALU.mult))
        A(lambda e: e.activation(out=mag[:], in_=lrdt[:], func=AF.Exp))
        D(lambda e: e.tensor_scalar(out=t1[:], in0=lidt[:], scalar1=1.0 / 16, scalar2=None, op0=ALU.mult))
        D(lambda e: e.tensor_scalar(out=t2[:], in0=lidt[:], scalar1=1.0 / 16, scalar2=float(np.pi / 2), op0=ALU.mult, op1=ALU.add))
        A(lambda e: e.activation(out=sn[:], in_=t1[:], func=AF.Sin))
        A(lambda e: e.activation(out=cs_[:], in_=t2[:], func=AF.Sin))
        for _ in range(4):
            D(lambda e: e.tensor_tensor(out=t1[:], in0=sn[:], in1=cs_[:], op=ALU.mult))
            D(lambda e: e.tensor_tensor(out=t2[:], in0=sn[:], in1=sn[:], op=ALU.mult))
            D(lambda e: e.tensor_tensor(out=cs_[:], in0=cs_[:], in1=cs_[:], op=ALU.mult))
            D(lambda e: e.tensor_tensor(out=cs_[:], in0=cs_[:], in1=t2[:], op=ALU.subtract))
            D(lambda e: e.tensor_scalar(out=sn[:], in0=t1[:], scalar1=2.0, scalar2=None, op0=ALU.mult))
        D(lambda e: e.tensor_tensor(out=are[:], in0=mag[:], in1=cs_[:], op=ALU.mult))
        D(lambda e: e.tensor_tensor(out=aim[:], in0=mag[:], in1=sn[:], op=ALU.mult))
        D(lambda e: e.tensor_tensor(out=den[:], in0=lr, in1=lr, op=ALU.mult))
        D(lambda e: e.tensor_tensor(out=t1[:], in0=li, in1=li, op=ALU.mult))
        D(lambda e: e.tensor_tensor(out=den[:], in0=den[:], in1=t1[:], op=ALU.add))
        D(lambda e: e.reciprocal(out=den[:], in_=den[:]))
        D(lambda e: e.tensor_scalar(out=am1[:], in0=are[:], scalar1=-1.0, scalar2=None, op0=ALU.add))
        D(lambda e: e.tensor_tensor(out=t1[:], in0=am1[:], in1=lr, op=ALU.mult))
        D(lambda e: e.tensor_tensor(out=t2[:], in0=aim[:], in1=li, op=ALU.mult))
        D(lambda e: e.tensor_tensor(out=qre[:], in0=t1[:], in1=t2[:], op=ALU.add))
        D(lambda e: e.tensor_tensor(out=qre[:], in0=qre[:], in1=den[:], op=ALU.mult))
        D(lambda e: e.tensor_tensor(out=t1[:], in0=aim[:], in1=lr, op=ALU.mult))
        D(lambda e: e.tensor_tensor(out=t2[:], in0=am1[:], in1=li, op=ALU.mult))
        D(lambda e: e.tensor_tensor(out=qim[:], in0=t1[:], in1=t2[:], op=ALU.subtract))
        D(lambda e: e.tensor_tensor(out=qim[:], in0=qim[:], in1=den[:], op=ALU.mult))
        A(lambda e: e.activation(out=im2[:], in_=lrdt[:], func=AF.Exp, scale=-2.0))
        D(lambda e: e.tensor_tensor(out=ire[:], in0=are[:], in1=im2[:], op=ALU.mult))
        D(lambda e: e.scalar_tensor_tensor(out=iim[:], in0=aim[:], scalar=-1.0, in1=im2[:], op0=ALU.mult, op1=ALU.mult))

        Epr = C.sb([128, 16, CS]); Epi = C.sb([128, 16, CS]); Enr = C.sb([128, 16, CS]); Eni = C.sb([128, 16, CS])
        T1 = C.sb([128, 16, CS]); T2 = C.sb([128, 16, CS]); T3 = C.sb([128, 16, CS]); T4 = C.sb([128, 16, CS])
        Lr = small(); Li = small()

        def table(Er, Ei, br, bi, keep_last=None):
            pr = small(); pi = small(); u1 = small(); u2 = small()
            D(lambda e: e.memset(Er[:, :, 0:1], 1.0))
            D(lambda e: e.memset(Ei[:, :, 0:1], 0.0))
            D(lambda e: e.tensor_copy(out=pr[:], in_=br[:]))
            D(lambda e: e.tensor_copy(out=pi[:], in_=bi[:]))
            n = 1
            while n < CS:
                prb = pr[:].unsqueeze(2).broadcast_to([128, 16, n])
                pib = pi[:].unsqueeze(2).broadcast_to([128, 16, n])
                D(lambda e, n=n, prb=prb: e.tensor_tensor(out=T1[:, :, 0:n], in0=Er[:, :, 0:n], in1=prb, op=ALU.mult))
                D(lambda e, n=n, pib=pib: e.tensor_tensor(out=T2[:, :, 0:n], in0=Ei[:, :, 0:n], in1=pib, op=ALU.mult))
                D(lambda e, n=n, pib=pib: e.tensor_tensor(out=T3[:, :, 0:n], in0=Er[:, :, 0:n], in1=pib, op=ALU.mult))
                D(lambda e, n=n, prb=prb: e.tensor_tensor(out=T4[:, :, 0:n], in0=Ei[:, :, 0:n], in1=prb, op=ALU.mult))
                D(lambda e, n=n: e.tensor_tensor(out=Er[:, :, n:2 * n], in0=T1[:, :, 0:n], in1=T2[:, :, 0:n], op=ALU.subtract))
                D(lambda e, n=n: e.tensor_tensor(out=Ei[:, :, n:2 * n], in0=T3[:, :, 0:n], in1=T4[:, :, 0:n], op=ALU.add))
                D(lambda e: e.tensor_tensor(out=u1[:], in0=pr[:], in1=pr[:], op=ALU.mult))
                D(lambda e: e.tensor_tensor(out=u2[:], in0=pi[:], in1=pi[:], op=ALU.mult))
                D(lambda e: e.tensor_tensor(out=pi[:], in0=pr[:], in1=pi[:], op=ALU.mult))
                D(lambda e: e.tensor_scalar(out=pi[:], in0=pi[:], scalar1=2.0, scalar2=None, op0=ALU.mult))
                D(lambda e: e.tensor_tensor(out=pr[:], in0=u1[:], in1=u2[:], op=ALU.subtract))
                n *= 2
            if keep_last is not None:
                D(lambda e: e.tensor_copy(out=keep_last[0][:], in_=pr[:]))
                D(lambda e: e.tensor_copy(out=keep_last[1][:], in_=pi[:]))

        table(Epr, Epi, are, aim, keep_last=(Lr, Li))
        table(Enr, Eni, ire, iim)
        qrb = qre[:].unsqueeze(2).broadcast_to([128, 16, CS]); qib = qim[:].unsqueeze(2).broadcast_to([128, 16, CS])
        D(lambda e: e.tensor_tensor(out=T1[:], in0=Enr[:], in1=qrb, op=ALU.mult))
        D(lambda e: e.tensor_tensor(out=T2[:], in0=Eni[:], in1=qib, op=ALU.mult))
        D(lambda e: e.tensor_tensor(out=T3[:], in0=Enr[:], in1=qib, op=ALU.mult))
        D(lambda e: e.tensor_tensor(out=T4[:], in0=Eni[:], in1=qrb, op=ALU.mult))
        D(lambda e: e.tensor_tensor(out=Enr[:], in0=T1[:], in1=T2[:], op=ALU.subtract))
        D(lambda e: e.tensor_tensor(out=Eni[:], in0=T3[:], in1=T4[:], op=ALU.add))

        u = C.sb([128, 4, TT]); xr = C.sb([128, 4, TT]); xi = C.sb([128, 4, TT]); cr = C.sb([128, 4, TT]); ci = C.sb([128, 4, TT])
        w1 = C.sb([128, TT]); w2_ = C.sb([128, TT]); w3 = C.sb([128, TT]); w4 = C.sb([128, TT])
        yb = C.sb([128, TT]); y2 = C.sb([128, TT]); sgm = C.sb([128, TT]); yo = C.sb([128, 4, TT])
        ones = C.sb([128, CS]); car_r = C.sb([128, 16]); car_i = C.sb([128, 16])
        c1 = small(4); c2 = small(4); c3 = small(4); c4 = small(4)
        pre = [C.ps([128, 512]) for _ in range(2)]; pim = [C.ps([128, 512]) for _ in range(2)]; psy = [C.ps([128, 512]) for _ in range(2)]
        t_pre = [Tok(), Tok()]; t_pim = [Tok(), Tok()]; t_psy = [Tok(), Tok()]
        kn = ['u', 'xr', 'xi', 'cr', 'ci', 'w1', 'w2', 'w3', 'w4', 'yb', 'y2', 'sgm', 'yo', 'car', 'c', 'ysd']
        k = {n: Tok(n) for n in kn}
        R.op('dve', lambda e: e.memset(ones[:], 1.0), writes=[tS])
        R.op('dve', lambda e: e.memset(car_r[:], 0.0), writes=[k['car']])
        R.op('dve', lambda e: e.memset(car_i[:], 0.0), writes=[k['car']])

        def v4(ap):
            return ap.rearrange("p (q s) -> p q s", s=CS)

        nmm = [0]
        for it in range(NT):
            t0 = it * TT
            for j in range(4):
                R.dma('sp', lambda e, t0=t0, j=j: e.dma_start(out=u[:, j, :], in_=zu[j, :, t0:t0 + TT]), writes=[k['u']])
            for ct in range(4):
                for j in range(4):
                    st = ct * 4 + j
                    b = nmm[0] % 2
                    nmm[0] += 1
                    R.op('pe', lambda e, b=b, st=st, ct=ct: e.matmul(pre[b][:, 0:TT], Bt[:, st, 0, :], u[:, ct, :], start=True, stop=True),
                         reads=[tP, k['u']], writes=[t_pre[b]])
                    R.op('pe', lambda e, b=b, st=st, ct=ct: e.matmul(pim[b][:, 0:TT], Bt[:, st, 1, :], u[:, ct, :], start=True, stop=True),
                         reads=[tP, k['u']], writes=[t_pim[b]])
                    enr = Enr[:, st, :].unsqueeze(1).broadcast_to([128, NQ, CS]); eni = Eni[:, st, :].unsqueeze(1).broadcast_to([128, NQ, CS])
                    R.op('dve', lambda e, b=b, enr=enr: e.tensor_tensor(out=v4(w1[:]), in0=v4(pre[b][:, 0:TT]), in1=enr, op=ALU.mult), reads=[t_pre[b], tS], writes=[k['w1']])
                    R.op('dve', lambda e, b=b, eni=eni: e.tensor_tensor(out=v4(w2_[:]), in0=v4(pim[b][:, 0:TT]), in1=eni, op=ALU.mult), reads=[t_pim[b], tS], writes=[k['w2']])
                    R.op('dve', lambda e, b=b, eni=eni: e.tensor_tensor(out=v4(w3[:]), in0=v4(pre[b][:, 0:TT]), in1=eni, op=ALU.mult), reads=[t_pre[b], tS], writes=[k['w3']])
                    R.op('dve', lambda e, b=b, enr=enr: e.tensor_tensor(out=v4(w4[:]), in0=v4(pim[b][:, 0:TT]), in1=enr, op=ALU.mult), reads=[t_pim[b], tS], writes=[k['w4']])
                    R.op('dve', lambda e, j=j: e.tensor_tensor(out=xr[:, j, :], in0=w1[:], in1=w2_[:], op=ALU.subtract), reads=[k['w1'], k['w2']], writes=[k['xr']])
                    R.op('dve', lambda e, j=j: e.tensor_tensor(out=xi[:, j, :], in0=w3[:], in1=w4[:], op=ALU.add), reads=[k['w3'], k['w4']], writes=[k['xi']])
                for q in range(NQ):
                    qs = slice(q * CS, (q + 1) * CS)
                    for j in range(4):
                        st = ct * 4 + j
                        R.op('dve', lambda e, j=j, qs=qs, st=st: e.tensor_tensor_scan(out=cr[:, j, qs], data0=ones[:], data1=xr[:, j, qs],
                                                                                   initial=car_r[:, st:st + 1], op0=ALU.mult, op1=ALU.add),
                             reads=[k['xr'], k['car'], tS], writes=[k['cr']])
                        R.op('dve', lambda e, j=j, qs=qs, st=st: e.tensor_tensor_scan(out=ci[:, j, qs], data0=ones[:], data1=xi[:, j, qs],
                                                                                   initial=car_i[:, st:st + 1], op0=ALU.mult, op1=ALU.add),
                             reads=[k['xi'], k['car'], tS], writes=[k['ci']])
                    last = q * CS + CS - 1
                    sl = slice(ct * 4, ct * 4 + 4)
                    R.op('dve', lambda e, last=last, sl=sl: e.tensor_tensor(out=c1[:], in0=cr[:, :, last], in1=Lr[:, sl], op=ALU.mult), reads=[k['cr'], tS], writes=[k['c']])
                    R.op('dve', lambda e, last=last, sl=sl: e.tensor_tensor(out=c2[:], in0=ci[:, :, last], in1=Li[:, sl], op=ALU.mult), reads=[k['ci'], tS], writes=[k['c']])
                    R.op('dve', lambda e, last=last, sl=sl: e.tensor_tensor(out=c3[:], in0=cr[:, :, last], in1=Li[:, sl], op=ALU.mult), reads=[k['cr'], tS], writes=[k['c']])
                    R.op('dve', lambda e, last=last, sl=sl: e.tensor_tensor(out=c4[:], in0=ci[:, :, last], in1=Lr[:, sl], op=ALU.mult), reads=[k['ci'], tS], writes=[k['c']])
                    R.op('dve', lambda e, sl=sl: e.tensor_tensor(out=car_r[:, sl], in0=c1[:], in1=c2[:], op=ALU.subtract), reads=[k['c']], writes=[k['car']])
                    R.op('dve', lambda e, sl=sl: e.tensor_tensor(out=car_i[:, sl], in0=c3[:], in1=c4[:], op=ALU.add), reads=[k['c']], writes=[k['car']])
                pb = ct % 2
                for j in range(4):
                    st = ct * 4 + j
                    epr = Epr[:, st, :].unsqueeze(1).broadcast_to([128, NQ, CS]); epi = Epi[:, st, :].unsqueeze(1).broadcast_to([128, NQ, CS])
                    R.op('dve', lambda e, j=j, epr=epr: e.tensor_tensor(out=v4(w1[:]), in0=v4(cr[:, j, :]), in1=epr, op=ALU.mult), reads=[k['cr'], tS], writes=[k['w1']])
                    R.op('dve', lambda e, j=j, epi=epi: e.tensor_tensor(out=v4(w2_[:]), in0=v4(ci[:, j, :]), in1=epi, op=ALU.mult), reads=[k['ci'], tS], writes=[k['w2']])
                    R.op('dve', lambda e, j=j, epi=epi: e.tensor_tensor(out=v4(w3[:]), in0=v4(cr[:, j, :]), in1=epi, op=ALU.mult), reads=[k['cr'], tS], writes=[k['w3']])
                    R.op('dve', lambda e, j=j, epr=epr: e.tensor_tensor(out=v4(w4[:]), in0=v4(ci[:, j, :]), in1=epr, op=ALU.mult), reads=[k['ci'], tS], writes=[k['w4']])
                    R.op('dve', lambda e, j=j: e.tensor_tensor(out=xr[:, j, :], in0=w1[:], in1=w2_[:], op=ALU.subtract), reads=[k['w1'], k['w2']], writes=[k['xr']])
                    R.op('dve', lambda e, j=j: e.tensor_tensor(out=xi[:, j, :], in0=w3[:], in1=w4[:], op=ALU.add), reads=[k['w3'], k['w4']], writes=[k['xi']])
                    R.op('pe', lambda e, pb=pb, st=st, j=j: e.matmul(psy[pb][:, 0:TT], Ct[:, st, 0, :], xr[:, j, :], start=(j == 0), stop=False),
                         reads=[tP, k['xr']], writes=[t_psy[pb]])
                    R.op('pe', lambda e, pb=pb, st=st, j=j: e.matmul(psy[pb][:, 0:TT], Ct[:, st, 1, :], xi[:, j, :], start=False, stop=(j == 3)),
                         reads=[tP, k['xi']], writes=[t_psy[pb]])
                R.op('dve', lambda e, pb=pb, ct=ct: e.scalar_tensor_tensor(out=yb[:], in0=u[:, ct, :], scalar=dsk[:, ct:ct + 1], in1=psy[pb][:, 0:TT],
                                                                           op0=ALU.mult, op1=ALU.add), reads=[k['u'], t_psy[pb], tP], writes=[k['yb']])
                R.op('dve', lambda e: e.tensor_tensor(out=y2[:], in0=yb[:], in1=yb[:], op=ALU.mult), reads=[k['yb']], writes=[k['y2']])
                R.op('dve', lambda e: e.tensor_scalar(out=y2[:], in0=y2[:], scalar1=0.044715, scalar2=1.0, op0=ALU.mult, op1=ALU.add), reads=[k['y2']], writes=[k['y2']])
                R.op('dve', lambda e: e.tensor_tensor(out=y2[:], in0=y2[:], in1=yb[:], op=ALU.mult), reads=[k['y2'], k['yb']], writes=[k['y2']])
                R.op('act', lambda e: e.activation(out=sgm[:], in_=y2[:], func=AF.Sigmoid, scale=1.5957691216057308), reads=[k['y2']], writes=[k['sgm']])
                R.op('dve', lambda e, ct=ct: e.tensor_tensor(out=yo[:, ct, :], in0=yb[:], in1=sgm[:], op=ALU.mult), reads=[k['yb'], k['sgm']], writes=[k['yo']])
            for j in range(4):
                R.dma('sp', lambda e, t0=t0, j=j: e.dma_start(out=ys[j, :, t0:t0 + TT], in_=yo[:, j, :]), reads=[k['yo']], writes=[k['ysd']])
        R.wait_all('sp', [k['ysd']])
        R.emit(nc, es)
    return nc


def s5_core_inputs(zu, lp, hg):
    T = zu.shape[-1]
    gs = slice(hg * 32, (hg + 1) * 32)
    out = {"zu": np.ascontiguousarray(zu[hg * 512:(hg + 1) * 512].reshape(4, 128, T))}

    def st_layout(a):
        return np.ascontiguousarray(a.reshape(16, 2, 64).transpose(1, 2, 0).reshape(128, 16))
    lam = np.zeros((128, 48), np.float32)
    lam[:, 0:16] = st_layout(lp['lam_re'][gs])
    lam[:, 16:32] = st_layout(lp['lam_im'][gs])
    lam[:, 32:48] = st_layout(np.repeat(lp['log_step'][gs][:, None], 64, axis=1))
    out["lam"] = lam
    Bt = np.zeros((128, 16, 2, 128), np.float32)
    Ct = np.zeros((128, 16, 2, 128), np.float32)
    for c, (bn, cn) in enumerate([('b_re', 'c_re'), ('b_im', 'c_im')]):
        Bm = lp[bn][gs]
        Cm = lp[cn][gs]
        for st in range(16):
            j = st % 4
            for gi in range(2):
                g = 2 * st + gi
                rows = slice(32 * j + gi * 16, 32 * j + gi * 16 + 16)
                cols = slice(gi * 64, gi * 64 + 64)
                Bt[rows, st, c, cols] = Bm[g].T
                Ct[cols, st, c, rows] = Cm[g].T
    out["Bt"] = Bt.reshape(128, -1)
    out["Ct"] = Ct.reshape(128, -1)
    out["dsk"] = _cols(lp['d_skip'][hg * 512:(hg + 1) * 512], 4)
    return out


class TP:
    def __init__(self, C, TT, wmax, nwb=4):
        self.C = C
        self.R = C.R
        self.TT = TT
        self.wb = [C.sb([128, wmax], BF16) for _ in range(nwb)]
        self.t_wb = [Tok() for _ in range(nwb)]
        self.nw = 0
        self.mp = [C.ps([128, 512]) for _ in range(4)]
        self.t_mp = [Tok() for _ in range(4)]
        self.nm = 0
        self.ssp = C.ps([128, 512])
        self.t_ssp = Tok()
        self.sq = [C.sb([128, TT]) for _ in range(2)]
        self.t_sq = [Tok(), Tok()]
        self.nsq = 0
        self.ones = C.sb([128, 128])
        self.t_c = Tok()
        self.rstd = C.sb([128, TT])
        self.t_rstd = Tok()
        self.R.op('pool', lambda e: e.memset(self.ones[:], 1.0), writes=[self.t_c])

    def next_mp(self):
        i = self.nm % 4
        self.nm += 1
        return self.mp[i], self.t_mp[i]

    def linear(self, act, t_act, KT, w_dram, nch, epilogue, cw=128):
        R, TT = self.R, self.TT
        for n in range(nch):
            i = self.nw % len(self.wb)
            self.nw += 1
            wbuf, tw = self.wb[i], self.t_wb[i]
            R.dma('pool', lambda e, wbuf=wbuf, n=n: e.dma_start(out=wbuf[:, 0:KT * cw], in_=w_dram[n]), writes=[tw])
            p, tp = self.next_mp()
            for kt in range(KT):
                R.op('pe', lambda e, p=p, wbuf=wbuf, kt=kt: e.matmul(p[0:cw, 0:TT], wbuf[:, kt * cw:(kt + 1) * cw], act(kt),
                                                                    start=(kt == 0), stop=(kt == KT - 1)),
                     reads=[tw] + list(t_act), writes=[tp])
            epilogue(n, p, tp)

    def sumsq(self, src, t_src, first, last):
        R, TT = self.R, self.TT
        i = self.nsq % 2
        self.nsq += 1
        sq, tsq = self.sq[i], self.t_sq[i]
        R.op('act', lambda e: e.activation(out=sq[:], in_=src, func=AF.Square), reads=list(t_src), writes=[tsq])
        R.op('pe', lambda e: e.matmul(self.ssp[:, 0:TT], self.ones[:], sq[:], start=first, stop=last), reads=[tsq, self.t_c], writes=[self.t_ssp])

    def finalize(self, D):
        R, TT = self.R, self.TT
        R.op('dve', lambda e: e.tensor_scalar(out=self.rstd[:], in0=self.ssp[:, 0:TT], scalar1=1.0 / D, scalar2=RMS_EPS, op0=ALU.mult, op1=ALU.add),
             reads=[self.t_ssp], writes=[self.t_rstd])
        R.op('act', lambda e: e.activation(out=self.rstd[:], in_=self.rstd[:], func=AF.Sqrt), reads=[self.t_rstd], writes=[self.t_rstd])
        R.op('dve', lambda e: e.reciprocal(out=self.rstd[:], in_=self.rstd[:]), reads=[self.t_rstd], writes=[self.t_rstd])


def build_p1(TOK, D=D_MODEL, NCH=68, TT=512):
    KT = D // 128
    NT = TOK // TT
    nc = bass.Bass("TRN2", target_bir_lowering=False)
    with ExitStack() as es:
        C = Ctx(nc, es)
        R = C.R
        hT = C.din("hT", [KT, 128, TOK])
        g_d = C.din("g", [128, KT])
        w_d = C.din("w", [NCH, 128, KT * 128])
        zT = C.dout("zT", [NCH, 128, TOK])
        tp_ = TP(C, TT, KT * 128)
        g = C.sb([128, KT]); tg = Tok()
        R.dma('sp', lambda e: e.dma_start(out=g[:], in_=g_d), writes=[tg])
        h = C.sb([128, KT, TT]); hn = C.sb([128, KT, TT], BF16)
        th = [Tok() for _ in range(KT)]; thn = Tok()
        stg = [C.sb([128, TT]) for _ in range(4)]; t_stg = [Tok() for _ in range(4)]
        tz = Tok()
        ns = [0]
        for it in range(NT):
            t0 = it * TT
            for kt in range(KT):
                R.dma('sp', lambda e, kt=kt, t0=t0: e.dma_start(out=h[:, kt, :], in_=hT[kt, :, t0:t0 + TT]), writes=[th[kt]])
            for kt in range(KT):
                tp_.sumsq(h[:, kt, :], [th[kt]], kt == 0, kt == KT - 1)
            tp_.finalize(D)
            for kt in range(KT):
                R.op('dve', lambda e, kt=kt: e.scalar_tensor_tensor(out=hn[:, kt, :], in0=h[:, kt, :], scalar=g[:, kt:kt + 1], in1=tp_.rstd[:],
                                                                    op0=ALU.mult, op1=ALU.mult), reads=[th[kt], tg, tp_.t_rstd], writes=[thn])

            def epi(n, p, tp, t0=t0):
                i = ns[0] % 4
                ns[0] += 1
                R.op('act', lambda e: e.activation(out=stg[i][:], in_=p[:, 0:TT], func=AF.Copy), reads=[tp], writes=[t_stg[i]])
                R.dma('sp', lambda e: e.dma_start(out=zT[n, :, t0:t0 + TT], in_=stg[i][:]), reads=[t_stg[i]], writes=[tz])
            tp_.linear(lambda kt: hn[:, kt, :], [thn], KT, w_d, NCH, epi)
        R.wait_all('sp', [tz])
        R.emit(nc, es)
    return nc


def tile_w(W, cw=128):
    K, N = W.shape
    KT, NB = K // 128, N // cw
    return np.ascontiguousarray(W.reshape(KT, 128, NB, cw).transpose(2, 1, 0, 3).reshape(NB, 128, KT * cw))


def build_p3(TOK, D=D_MODEL, HW=RW, DFF=D_FF, PL=PLE, TT=512, HBC=16, stages=3):
    KT = D // 128
    KH = HW // 128
    KM = 2 * KH
    NF = DFF // 128
    NBLK = NF // HBC
    KP = PL // 128
    NT = TOK // TT
    nc = bass.Bass("TRN2", target_bir_lowering=False)
    with ExitStack() as es:
        C = Ctx(nc, es)
        R = C.R
        yr_d = C.din("yr", [KH, 128, TOK]); ys_d = C.din("ys", [KH, 128, TOK]); h_d = C.din("hT", [KT, 128, TOK]); p_d = C.din("pT", [KP, 128, TOK])
        vec_d = C.din("vec", [128, 5 * KT + KH])
        wglu_d = C.din("w_glu", [KH, 128, KH * 128]); wout_d = C.din("w_out", [KT, 128, KM * 128])
        wff1_d = C.din("w_ff1", [NF, 128, KT * 128]); wff2_d = C.din("w_ff2", [NBLK * KT, 128, HBC * 128])
        wple_d = C.din("w_ple", [KT, 128, KP * 128]); wgate_d = C.din("w_gate", [KT, 128, KT * 128])
        ho_d = C.dout("hoT", [KT, 128, TOK])
        tp_ = TP(C, TT, max(KT, KM, HBC) * 128)
        vec = C.sb([128, 5 * KT + KH]); tv = Tok()
        R.dma('sp', lambda e: e.dma_start(out=vec[:], in_=vec_d), writes=[tv])

        def gv(i, n):
            return vec[:, i * KT + n:i * KT + n + 1]

        def bglu(n):
            return vec[:, 5 * KT + n:5 * KT + n + 1]
        work = C.sb([128, KT, TT]); t_work = [Tok() for _ in range(KT)]
        A = C.sb([128, max(KT, KM), TT], BF16); t_A = Tok()
        B = [C.sb([128, HBC, TT], BF16) for _ in range(2)]; t_B = [Tok(), Tok()]
        hst = [C.sb([128, TT]) for _ in range(4)]; t_hst = [Tok() for _ in range(4)]
        tmp = [C.sb([128, TT]) for _ in range(2)]; t_tmp = [Tok(), Tok()]
        ysf = [C.sb([128, TT]) for _ in range(2)]; t_ysf = [Tok(), Tok()]
        pT = C.sb([128, KP, TT], BF16); t_pT = Tok()
        ep = [C.ps([128, 512]) for _ in range(2)]; t_ep = [Tok(), Tok()]
        t_ho = {}
        cnt = {'hst': 0, 'tmp': 0, 'ysf': 0, 'ep': 0}

        def rot(name, bufs, toks):
            i = cnt[name] % len(bufs)
            cnt[name] += 1
            return bufs[i], toks[i]

        def do_tile(it):
            t0 = it * TT
            ts = slice(t0, t0 + TT)
            for n in range(KT):
                t_ho[(it, n)] = Tok()

            def residual(src_d, gi, final, it=it, ts=ts):
                for n in range(KT):
                    hb, thb = rot('hst', hst, t_hst)
                    tb, ttb = rot('tmp', tmp, t_tmp)
                    R.dma('sp', lambda e, hb=hb, n=n: e.dma_start(out=hb[:], in_=src_d[n, :, ts]), reads=[t_ho[(it, n)]], writes=[thb])
                    R.op('dve', lambda e, tb=tb, n=n: e.scalar_tensor_tensor(out=tb[:], in0=work[:, n, :], scalar=gv(gi, n), in1=tp_.rstd[:],
                                                                             op0=ALU.mult, op1=ALU.mult),
                         reads=[t_work[n], tv, tp_.t_rstd], writes=[ttb])
                    if final:
                        R.op('dve', lambda e, hb=hb, tb=tb: e.tensor_tensor(out=hb[:], in0=hb[:], in1=tb[:], op=ALU.add), reads=[thb, ttb], writes=[thb])
                        R.dma('sp', lambda e, hb=hb, n=n: e.dma_start(out=ho_d[n, :, ts], in_=hb[:]), reads=[thb], writes=[t_ho[(it, n)]])
                    else:
                        R.op('dve', lambda e, hb=hb, tb=tb, n=n: e.tensor_tensor(out=work[:, n, :], in0=hb[:], in1=tb[:], op=ALU.add),
                             reads=[thb, ttb], writes=[t_work[n]])
                        R.dma('sp', lambda e, n=n: e.dma_start(out=ho_d[n, :, ts], in_=work[:, n, :]), reads=[t_work[n]], writes=[t_ho[(it, n)]])
                        tp_.sumsq(work[:, n, :], [t_work[n]], n == 0, n == KT - 1)

            def make_hn(gi):
                for n in range(KT):
                    R.op('dve', lambda e, n=n: e.scalar_tensor_tensor(out=A[:, n, :], in0=work[:, n, :], scalar=gv(gi, n), in1=tp_.rstd[:],
                                                                      op0=ALU.mult, op1=ALU.mult),
                         reads=[t_work[n], tv, tp_.t_rstd], writes=[t_A])

            for n in range(KH):
                R.dma('pool', lambda e, n=n: e.dma_start(out=A[:, n, :], in_=yr_d[n, :, ts]), writes=[t_A])
                R.dma('pool', lambda e, n=n: e.dma_start(out=B[0][:, n, :], in_=ys_d[n, :, ts]), writes=[t_B[0]])

            def epi_glu(n, p, tp):
                tb, ttb = rot('tmp', tmp, t_tmp)
                yb, tyb = rot('ysf', ysf, t_ysf)
                R.dma('sp', lambda e, yb=yb: e.dma_start(out=yb[:], in_=ys_d[n, :, ts]), writes=[tyb])
                R.op('act', lambda e, tb=tb: e.activation(out=tb[:], in_=p[:, 0:TT], func=AF.Sigmoid, bias=bglu(n)), reads=[tp, tv], writes=[ttb])
                R.op('dve', lambda e, tb=tb, yb=yb: e.tensor_tensor(out=A[:, KH + n, :], in0=yb[:], in1=tb[:], op=ALU.mult), reads=[ttb, tyb], writes=[t_A])
            tp_.linear(lambda kt: B[0][:, kt, :], [t_B[0]], KH, wglu_d, KH, epi_glu)

            def epi_out(n, p, tp):
                R.op('act', lambda e: e.activation(out=work[:, n, :], in_=p[:, 0:TT], func=AF.Copy), reads=[tp], writes=[t_work[n]])
                tp_.sumsq(p[:, 0:TT], [tp], n == 0, n == KT - 1)
            tp_.linear(lambda kt: A[:, kt, :], [t_A], KM, wout_d, KT, epi_out)
            tp_.finalize(D)
            residual(h_d, 0, False)
            tp_.finalize(D)
            make_hn(1)

            if stages < 2:
                return
            for blk in range(NBLK):
                Bb, tBb = B[blk % 2], t_B[blk % 2]

                def epi_ff1(n, p, tp, Bb=Bb, tBb=tBb):
                    tb, ttb = rot('tmp', tmp, t_tmp)
                    R.op('act', lambda e, tb=tb: e.activation(out=tb[:], in_=p[:, 0:TT], func=AF.Relu), reads=[tp], writes=[ttb])
                    R.op('dve', lambda e, tb=tb: e.tensor_tensor(out=Bb[:, n, :], in0=tb[:], in1=tb[:], op=ALU.mult), reads=[ttb], writes=[tBb])
                tp_.linear(lambda kt: A[:, kt, :], [t_A], KT, wff1_d[blk * HBC:(blk + 1) * HBC], HBC, epi_ff1)

                def epi_ff2(n, p, tp, blk=blk):
                    if blk == 0:
                        R.op('act', lambda e: e.activation(out=work[:, n, :], in_=p[:, 0:TT], func=AF.Copy), reads=[tp], writes=[t_work[n]])
                    else:
                        R.op('dve', lambda e: e.tensor_tensor(out=work[:, n, :], in0=work[:, n, :], in1=p[:, 0:TT], op=ALU.add),
                             reads=[tp, t_work[n]], writes=[t_work[n]])
                tp_.linear(lambda kt, Bb=Bb: Bb[:, kt, :], [tBb], HBC, wff2_d[blk * KT:(blk + 1) * KT], KT, epi_ff2)
            for n in range(KT):
                tp_.sumsq(work[:, n, :], [t_work[n]], n == 0, n == KT - 1)
            tp_.finalize(D)
            residual(ho_d, 2, False)
            tp_.finalize(D)
            make_hn(3)

            if stages < 3:
                return
            for kp in range(KP):
                R.dma('pool', lambda e, kp=kp: e.dma_start(out=pT[:, kp, :], in_=p_d[kp, :, ts]), writes=[t_pT])

            def epi_gate(n, p, tp):
                i = tp_.nw % len(tp_.wb)
                tp_.nw += 1
                wbuf, tw = tp_.wb[i], tp_.t_wb[i]
                R.dma('pool', lambda e: e.dma_start(out=wbuf[:, 0:KP * 128], in_=wple_d[n]), writes=[tw])
                pe_, tpe = rot('ep', ep, t_ep)
                for kp in range(KP):
                    R.op('pe', lambda e, kp=kp: e.matmul(pe_[:, 0:TT], wbuf[:, kp * 128:(kp + 1) * 128], pT[:, kp, :], start=(kp == 0), stop=(kp == KP - 1)),
                         reads=[tw, t_pT], writes=[tpe])
                tb, ttb = rot('tmp', tmp, t_tmp)
                R.op('act', lambda e, tb=tb: e.activation(out=tb[:], in_=p[:, 0:TT], func=AF.Sigmoid), reads=[tp], writes=[ttb])
                R.op('dve', lambda e, tb=tb: e.tensor_tensor(out=work[:, n, :], in0=tb[:], in1=pe_[:, 0:TT], op=ALU.mult), reads=[ttb, tpe], writes=[t_work[n]])
                tp_.sumsq(work[:, n, :], [t_work[n]], n == 0, n == KT - 1)
            tp_.linear(lambda kt: A[:, kt, :], [t_A], KT, wgate_d, KT, epi_gate)
            tp_.finalize(D)
            residual(ho_d, 4, True)

        for it in range(NT):
            do_tile(it)
        R.wait_all('sp', [t_ho[(NT - 1, n)] for n in range(KT)] + [t_ho[(it_, n)] for it_ in range(NT) for n in range(KT)])
        R.emit(nc, es)
    return nc


def tile_w2(W, HBC):
    DFF, D = W.shape
    NBLK = DFF // (128 * HBC)
    KT = D // 128
    return np.ascontiguousarray(W.reshape(NBLK, HBC, 128, KT, 128).transpose(0, 3, 2, 1, 4).reshape(NBLK * KT, 128, HBC * 128))


_PROG = {}


def _prog(name, fn):
    if name not in _PROG:
        _PROG[name] = fn()
    return _PROG[name]


def _run(nc, in_maps):
    res = run_bass_kernel_spmd(nc, in_maps, core_ids=list(range(NCORES)))
    return res.results


def kernel(**inp):
    f32 = np.float32
    x = np.asarray(inp["x"], f32)
    B, S, D = x.shape
    SEG = S * B // NCORES
    NSEG = S // SEG
    KT = D // 128
    hT = []
    for c in range(NCORES):
        b, j = c // NSEG, c % NSEG
        hT.append(np.ascontiguousarray(x[b, j * SEG:(j + 1) * SEG].T).reshape(KT, 128, SEG))
    p_all = np.asarray(inp["p"], f32)
    for i in range(DEPTH):
        L = {k: np.asarray(v[i], f32) for k, v in inp.items() if k not in ("x", "p")}
        w_in = L["w_in"]
        wp = np.zeros((D, 68 * 128), f32)
        wp[:, 0:6144] = w_in[:, 0:6144]
        wp[:, 6144:6144 + 96] = w_in[:, 6144:6240]
        wp[:, 6272:6272 + 96] = w_in[:, 6240:6336]
        wp[:, 6400:6656] = w_in[:, 6336:6592]
        wp[:, 6656:8704] = w_in[:, 6592:8640]
        wt = tile_w(wp)
        del wp
        g1 = _cols(L["g_mix_pre"], KT)
        nc1 = _prog("p1", lambda: build_p1(SEG))
        r1 = _run(nc1, [{"hT": hT[c], "g": g1, "w": wt} for c in range(NCORES)])
        del wt
        zb = []
        for b in range(B):
            zb.append(np.concatenate([r1[b * NSEG + j]["zT"].reshape(68 * 128, SEG) for j in range(NSEG)], axis=1))
        del r1
        ims_r, ims_s = [], []
        for c in range(NCORES):
            b, hg = c // 4, c % 4
            z = zb[b]
            ims_r.append(rwkv_core_inputs(z[0:2048], z[2048:4096], z[4096:6144], z[6144:6240], z[6272:6368], z[6400:6656], L, hg))
            ims_s.append(s5_core_inputs(z[6656:8704], L, hg))
        del zb
        nc2 = _prog("rwkv", lambda: build_rwkv(S))
        r2 = _run(nc2, ims_r)
        del ims_r
        nc3 = _prog("s5", lambda: build_s5(S))
        r3 = _run(nc3, ims_s)
        del ims_s
        vec = np.ascontiguousarray(np.concatenate(
            [_cols(L[k], KT) for k in ("g_mix_post", "g_ffn_pre", "g_ffn_post", "g_ple_gate", "g_ple_post")] + [_cols(L["b_glu"], 16)], axis=1))
        wd = {"w_glu": tile_w(L["w_glu"]), "w_out": tile_w(L["w_out"]), "w_ff1": tile_w(L["w_ff1"]), "w_ff2": tile_w2(L["w_ff2"], 16),
              "w_ple": tile_w(L["w_ple"]), "w_gate": tile_w(L["w_ple_gate"])}
        ims = []
        for c in range(NCORES):
            b, j = c // NSEG, c % NSEG
            sl = slice(j * SEG, (j + 1) * SEG)
            yr = np.ascontiguousarray(np.concatenate([r2[b * 4 + hg]["yr"][:, :, sl] for hg in range(4)], axis=0))
            ys = np.ascontiguousarray(np.concatenate([r3[b * 4 + hg]["ys"][:, :, sl] for hg in range(4)], axis=0))
            pT = np.ascontiguousarray(p_all[i, b, sl].T).reshape(PLE // 128, 128, SEG)
            d = {"yr": yr, "ys": ys, "hT": hT[c], "pT": pT, "vec": vec}
            d.update(wd)
            ims.append(d)
        del r2, r3
        nc4 = _prog("p3", lambda: build_p3(SEG))
        r4 = _run(nc4, ims)
        del ims, wd
        hT = [r4[c]["hoT"] for c in range(NCORES)]
        del r4
    out = np.empty((B, S, D), f32)
    for c in range(NCORES):
        b, j = c // NSEG, c % NSEG
        out[b, j * SEG:(j + 1) * SEG] = hT[c].reshape(D, SEG).T
    return out
```
